# Optimizing a Trainium2 kernel written in Bass

```python
import math
import jax, jax.numpy as jnp
from jax import lax
import numpy as np

D_MODEL = 1024
BATCH = 1
SEQ = 16384
DEPTH = 4

A_HEADS = 8
A_HEAD_DIM = 64
A_QK = A_HEADS * 2 * A_HEAD_DIM
A_V = A_HEADS * 2 * A_HEAD_DIM
Q_BLOCK = 128
M_HEADS = 4
M_INNER = D_MODEL
M_HEAD_DIM = M_INNER // M_HEADS
CONV_WIDTH = 4
CHUNK = 64
NUM_BUCKETS = 32
MAX_DISTANCE = 128
D_FF = 4 * D_MODEL
N_IN = 2 * A_QK + A_V + 4 * M_INNER + 2 * M_HEADS + 2 * D_MODEL
EPS = 1e-6

kernel_name = "hybrid_diffattn_mlstm_gated_block"


def rms_norm(x, g):
    xf = x.astype(jnp.float32)
    y = xf * lax.rsqrt(jnp.mean(xf * xf, axis=-1, keepdims=True) + EPS)
    return (y * g.astype(jnp.float32)).astype(x.dtype)


def t5_bucket(dist):
    max_exact = NUM_BUCKETS // 2
    nf = jnp.maximum(dist, 1).astype(jnp.float32)
    large = max_exact + (jnp.log(nf / max_exact) / math.log(MAX_DISTANCE / max_exact)
                         * (NUM_BUCKETS - max_exact)).astype(jnp.int32)
    large = jnp.minimum(large, NUM_BUCKETS - 1)
    return jnp.where(dist < max_exact, dist, large)


def causal_conv(u, w, b):
    s = u.shape[1]
    up = jnp.pad(u, ((0, 0), (CONV_WIDTH - 1, 0), (0, 0)))
    y = b
    for j in range(CONV_WIDTH):
        y = y + w[j] * up[:, j:j + s]
    return y


def diff_attention(q1, q2, k1, k2, v, lam, bias_by_dist):
    b, h, s, dh = q1.shape
    nb = s // Q_BLOCK
    scale = A_HEAD_DIM ** -0.5
    kpos = jnp.arange(s)

    def block(args):
        q1b, q2b, start = args
        qpos = start + jnp.arange(Q_BLOCK)
        dist = qpos[:, None] - kpos[None, :]
        causal = dist >= 0
        bias = jnp.take(bias_by_dist, jnp.clip(dist, 0, s - 1), axis=1)[None]

        def probs(qb, kk):
            logits = jnp.einsum('bhqd,bhkd->bhqk', qb, kk).astype(jnp.float32) * scale + bias
            logits = jnp.where(causal, logits, -jnp.inf)
            return jax.nn.softmax(logits, axis=-1)

        p = probs(q1b, k1) - lam * probs(q2b, k2)
        return jnp.einsum('bhqk,bhkd->bhqd', p.astype(v.dtype), v)

    to_blocks = lambda t: t.reshape(b, h, nb, Q_BLOCK, dh).transpose(2, 0, 1, 3, 4)
    starts = jnp.arange(nb) * Q_BLOCK
    out = lax.map(block, (to_blocks(q1), to_blocks(q2), starts))
    return out.transpose(1, 2, 0, 3, 4).reshape(b, h, s, v.shape[-1])


def mlstm_chunkwise(q, k, v, i_pre, f_pre):
    out_dtype = q.dtype
    b, nh, s, dh = q.shape
    nc = s // CHUNK
    qf = q.astype(jnp.float32)
    kf = k.astype(jnp.float32) * (dh ** -0.5)
    vf = v.astype(jnp.float32)
    ig = i_pre.astype(jnp.float32)
    logf = jax.nn.log_sigmoid(f_pre.astype(jnp.float32))
    chunk4 = lambda t: jnp.moveaxis(t.reshape(b, nh, nc, CHUNK, dh), 2, 0)
    chunk3 = lambda t: jnp.moveaxis(t.reshape(b, nh, nc, CHUNK), 2, 0)
    tril = jnp.tril(jnp.ones((CHUNK, CHUNK), dtype=bool))

    def step(carry, inp):
        cmat, nvec, m = carry
        qc, kc, vc, ic, lfc = inp
        bcum = jnp.cumsum(lfc, axis=-1)
        dmat = bcum[..., :, None] - bcum[..., None, :] + ic[..., None, :]
        dmat = jnp.where(tril, dmat, -jnp.inf)
        inter = bcum + m[..., None]
        m_t = jnp.maximum(jnp.max(dmat, axis=-1), inter)
        scores = jnp.einsum('bhtd,bhsd->bhts', qc, kc) * jnp.exp(dmat - m_t[..., None])
        a_inter = jnp.exp(inter - m_t)
        num = (jnp.einsum('bhts,bhsd->bhtd', scores, vc)
               + a_inter[..., None] * jnp.einsum('bhed,bhtd->bhte', cmat, qc))
        den = jnp.sum(scores, axis=-1) + a_inter * jnp.einsum('bhd,bhtd->bht', nvec, qc)
        h = num / jnp.maximum(jnp.abs(den), jnp.exp(-m_t))[..., None]
        b_last = bcum[..., -1]
        g = b_last[..., None] - bcum + ic
        m_new = jnp.maximum(b_last + m, jnp.max(g, axis=-1))
        wk = jnp.exp(g - m_new[..., None])
        decay = jnp.exp(b_last + m - m_new)
        c_new = decay[..., None, None] * cmat + jnp.einsum('bhs,bhse,bhsd->bhed', wk, vc, kc)
        n_new = decay[..., None] * nvec + jnp.einsum('bhs,bhsd->bhd', wk, kc)
        return (c_new, n_new, m_new), h

    init = (jnp.zeros((b, nh, dh, dh), jnp.float32),
            jnp.zeros((b, nh, dh), jnp.float32),
            jnp.zeros((b, nh), jnp.float32))
    _, hs = lax.scan(step, init, (chunk4(qf), chunk4(kf), chunk4(vf), chunk3(ig), chunk3(logf)))
    return jnp.moveaxis(hs, 0, 2).reshape(b, nh, s, dh).astype(out_dtype)


def setup_inputs(seed: int = 0) -> dict:
    key = jax.random.key(seed)
    ks = jax.random.split(key, 24)
    nrm = lambda k, shape, sc: jax.random.normal(k, shape, jnp.float32) * sc
    gain = lambda k, shape: 1.0 + 0.02 * jax.random.normal(k, shape, jnp.float32)
    return {
        "x": nrm(ks[0], (BATCH, SEQ, D_MODEL), 1.0),
        "c": nrm(ks[1], (BATCH, D_MODEL), 1.0),
        "w_ada": nrm(ks[2], (DEPTH, D_MODEL, 6 * D_MODEL), 0.5 * D_MODEL ** -0.5),
        "b_ada": nrm(ks[3], (DEPTH, 6 * D_MODEL), 0.02),
        "norm_mix_g": gain(ks[4], (DEPTH, D_MODEL)),
        "norm_ffn_g": gain(ks[5], (DEPTH, D_MODEL)),
        "w_in": nrm(ks[6], (DEPTH, D_MODEL, N_IN), D_MODEL ** -0.5),
        "b_igate": nrm(ks[7], (DEPTH, M_HEADS), 0.1),
        "b_fgate": jnp.linspace(3.0, 6.0, M_HEADS, dtype=jnp.float32)[None, :] + nrm(ks[8], (DEPTH, M_HEADS), 0.1),
        "qn_g": gain(ks[9], (DEPTH, A_HEAD_DIM)),
        "kn_g": gain(ks[10], (DEPTH, A_HEAD_DIM)),
        "lam_qk": nrm(ks[11], (DEPTH, 4, A_HEAD_DIM), 0.1),
        "subln_g": gain(ks[12], (DEPTH, 2 * A_HEAD_DIM)),
        "rel_table": nrm(ks[13], (NUM_BUCKETS, A_HEADS), 0.5),
        "conv_w": nrm(ks[14], (DEPTH, CONV_WIDTH, 2 * M_INNER), CONV_WIDTH ** -0.5),
        "conv_b": nrm(ks[15], (DEPTH, 2 * M_INNER), 0.02),
        "mhn_g": gain(ks[16], (DEPTH, M_HEAD_DIM)),
        "w_a": nrm(ks[17], (DEPTH, A_V, D_MODEL), A_V ** -0.5),
        "w_m": nrm(ks[18], (DEPTH, M_INNER, D_MODEL), M_INNER ** -0.5),
        "w_out": nrm(ks[19], (DEPTH, D_MODEL, D_MODEL), D_MODEL ** -0.5),
        "w_ff1": nrm(ks[20], (DEPTH, D_MODEL, D_FF), D_MODEL ** -0.5),
        "w_ff2": nrm(ks[21], (DEPTH, D_FF, D_MODEL), D_FF ** -0.5),
    }


def reference(x, c, w_ada, b_ada, norm_mix_g, norm_ffn_g, w_in, b_igate, b_fgate, qn_g, kn_g,
              lam_qk, subln_g, rel_table, conv_w, conv_b, mhn_g, w_a, w_m, w_out, w_ff1, w_ff2):
    b, s, _ = x.shape
    bias_by_dist = rel_table[t5_bucket(jnp.arange(s))].T.astype(jnp.float32)
    cond = jax.nn.silu(c)
    sizes = [A_QK, A_QK, A_V, M_INNER, M_INNER, M_INNER, M_INNER, M_HEADS, M_HEADS, D_MODEL, D_MODEL]
    points = []
    acc = 0
    for sz in sizes[:-1]:
        acc += sz
        points.append(acc)
    m_heads = lambda t: t.reshape(b, s, M_HEADS, M_HEAD_DIM).transpose(0, 2, 1, 3)

    for l in range(DEPTH):
        lam_init = 0.8 - 0.6 * math.exp(-0.3 * l)
        mod = cond @ w_ada[l] + b_ada[l]
        sh_a, sc_a, gt_a, sh_f, sc_f, gt_f = jnp.split(mod[:, None, :], 6, axis=-1)

        h = rms_norm(x, norm_mix_g[l]) * (1.0 + sc_a) + sh_a
        proj = h @ w_in[l]
        aq, ak, av, mq, mk, mv, mo, mi, mf, ga, gm = jnp.split(proj, points, axis=-1)

        aq = rms_norm(aq.reshape(b, s, A_HEADS, 2, A_HEAD_DIM).transpose(3, 0, 2, 1, 4), qn_g[l])
        ak = rms_norm(ak.reshape(b, s, A_HEADS, 2, A_HEAD_DIM).transpose(3, 0, 2, 1, 4), kn_g[l])
        av = av.reshape(b, s, A_HEADS, 2 * A_HEAD_DIM).transpose(0, 2, 1, 3)
        lq = lam_qk[l].astype(jnp.float32)
        lam = jnp.exp(jnp.sum(lq[0] * lq[1])) - jnp.exp(jnp.sum(lq[2] * lq[3])) + lam_init
        ya = diff_attention(aq[0], aq[1], ak[0], ak[1], av, lam, bias_by_dist)
        ya = rms_norm(ya, subln_g[l]) * (1.0 - lam_init)
        ya = ya.transpose(0, 2, 1, 3).reshape(b, s, A_V)

        qk_m = jax.nn.silu(causal_conv(jnp.concatenate([mq, mk], axis=-1), conv_w[l], conv_b[l]))
        mq, mk = jnp.split(qk_m, 2, axis=-1)
        hm = mlstm_chunkwise(m_heads(mq), m_heads(mk), m_heads(mv),
                             (mi + b_igate[l]).transpose(0, 2, 1), (mf + b_fgate[l]).transpose(0, 2, 1))
        hm = rms_norm(hm, mhn_g[l]).transpose(0, 2, 1, 3).reshape(b, s, M_INNER)
        ym = jax.nn.sigmoid(mo) * hm

        merged = jax.nn.sigmoid(ga) * (ya @ w_a[l]) + jax.nn.sigmoid(gm) * (ym @ w_m[l])
        x = x + gt_a * (merged @ w_out[l])

        h = rms_norm(x, norm_ffn_g[l]) * (1.0 + sc_f) + sh_f
        x = x + gt_f * (jnp.square(jax.nn.relu(h @ w_ff1[l])) @ w_ff2[l])
    return x
```

```python
import numpy as np
import concourse.bass as bass
import concourse.mybir as mybir
from concourse.bass_utils import run_bass_kernel_spmd
from contextlib import ExitStack

F32 = mybir.dt.float32
BF16 = mybir.dt.bfloat16
AF = mybir.ActivationFunctionType
ALU = mybir.AluOpType
AX = mybir.AxisListType

ENGS = ("sync", "scalar", "vector", "gpsimd", "tensor")


class U:
    __slots__ = ("name", "w", "r", "sem", "cnt", "lastdma")

    def __init__(self, name):
        self.name = name
        self.w = None
        self.r = []
        self.sem = None
        self.cnt = 0
        self.lastdma = None


class Op:
    __slots__ = ("eng", "fn", "deps", "pos", "is_dma", "unit", "cnt", "sig", "sigval", "name")


class Sched:
    def __init__(self, nc, es):
        self.nc = nc
        self.es = es
        self.ops = []
        self.streams = {e: [] for e in ENGS}
        self.nsb = 0

    def sb(self, shape, dt, name=None):
        self.nsb += 1
        return self.es.enter_context(self.nc.sbuf_tensor("s_" + (name or f"sb{self.nsb}"), list(shape), dt))

    def ps(self, shape, dt, name=None):
        self.nsb += 1
        return self.es.enter_context(self.nc.psum_tensor("p_" + (name or f"ps{self.nsb}"), list(shape), dt))

    def unit(self, name="u"):
        return U(name)

    def _rec(self, eng, fn, r, w, is_dma, name):
        op = Op()
        op.eng = eng
        op.fn = fn
        op.is_dma = is_dma
        op.sig = False
        op.sigval = 0
        op.name = name
        op.unit = None
        op.cnt = 0
        deps = {}
        for u in r:
            if u.w is not None:
                deps[id(u.w)] = (u.w, "raw", u)
        for u in w:
            if u.w is not None and id(u.w) not in deps:
                deps[id(u.w)] = (u.w, "waw", u)
            for rd in u.r:
                if id(rd) not in deps:
                    deps[id(rd)] = (rd, "war", u)
        op.deps = list(deps.values())
        if is_dma:
            du = w[0]
            op.unit = du
            du.cnt += 16
            op.cnt = du.cnt
        for u in r:
            u.r.append(op)
        for u in w:
            u.w = op
            u.r = []
        op.pos = len(self.streams[eng])
        self.streams[eng].append(op)
        self.ops.append(op)
        return op

    def op(self, eng, fn, r=(), w=(), name=""):
        return self._rec(eng, fn, list(r), list(w), False, name)

    def dma(self, eng, out, in_, r=(), w=(), name="", **kw):
        def fn(e, out=out, in_=in_, kw=kw):
            return e.dma_start(out=out, in_=in_, **kw)
        return self._rec(eng, fn, list(r), list(w), True, name)

    def finish(self, units, eng="sync"):
        return self._rec(eng, None, list(units), [], False, "finish")

    def emit(self):
        nc = self.nc
        es = self.es
        esem = {e: es.enter_context(nc.semaphore(f"s_{e}")) for e in ENGS}
        observed = {e: {} for e in ENGS}
        waits = {}
        for op in self.ops:
            wl = {}
            ob = observed[op.eng]
            for (d, kind, u) in op.deps:
                if d.is_dma:
                    du = d.unit
                    if du.sem is None:
                        du.sem = es.enter_context(nc.semaphore(f"d_{du.name}_{id(du) % 100000}"))
                    key = ("dma", id(du))
                    val = d.cnt
                    if ob.get(key, 0) >= val:
                        continue
                    ob[key] = val
                    wl[key] = (du.sem, max(val, wl.get(key, (None, 0))[1]))
                else:
                    if d.eng == op.eng:
                        if op.eng == "tensor" or kind != "raw":
                            continue
                        if op.is_dma or d.fn is None:
                            pass
                    key = ("eng", d.eng)
                    if ob.get(key, -1) >= d.pos:
                        continue
                    ob[key] = d.pos
                    prev = wl.get(key)
                    if prev is None or prev[1].pos < d.pos:
                        wl[key] = ("eng", d)
                    d.sig = True
            waits[id(op)] = wl
        for e in ENGS:
            c = 0
            for op in self.streams[e]:
                if op.sig and not op.is_dma:
                    c += 1
                    op.sigval = c
        self.nsig = {e: sum(1 for o in self.streams[e] if o.sig) for e in ENGS}

        def run_stream(ename, eng):
            for op in self.streams[ename]:
                for key, v in waits[id(op)].items():
                    if key[0] == "dma":
                        eng.wait_ge(v[0], v[1])
                    else:
                        d = v[1]
                        eng.wait_ge(esem[d.eng], d.sigval)
                if op.fn is None:
                    continue
                inst = op.fn(eng)
                if op.is_dma:
                    inst.then_inc(op.unit.sem, 16)
                elif op.sig:
                    inst.then_inc(esem[ename], 1)

        for op in self.ops:
            if op.is_dma and op.unit.sem is None:
                op.unit.sem = es.enter_context(nc.semaphore(f"d_{op.unit.name}_{id(op.unit) % 100000}"))

        with nc.Block() as block:
            @block.sync
            def _(e):
                run_stream("sync", e)

            @block.scalar
            def _(e):
                run_stream("scalar", e)

            @block.vector
            def _(e):
                run_stream("vector", e)

            @block.gpsimd
            def _(e):
                run_stream("gpsimd", e)

            @block.tensor
            def _(e):
                run_stream("tensor", e)


EPS = 1e-6
D = 1024
NIN = 9224


class K:
    pass


def din(nc, n, s, d=F32):
    return nc.dram_tensor(n, list(s), d, kind="ExternalInput").ap()


def dout(nc, n, s, d=F32):
    return nc.dram_tensor(n, list(s), d, kind="ExternalOutput").ap()


def mk_common(S):
    k = K()
    k.ones = S.sb([128, 128], F32, "ones")
    k.uones = S.unit("ones")
    S.op("gpsimd", lambda e: e.memset(k.ones[:], 1.0), w=[k.uones])
    k.pb = [S.ps([128, 512], F32, f"pb{i}") for i in range(8)]
    k.upb = [S.unit(f"pb{i}") for i in range(8)]
    k.rr = {}
    return k


def rot(k, name, n):
    i = k.rr.get(name, 0)
    k.rr[name] = i + 1
    return i % n


def mk_blockdiag(S, k, grp):
    t = S.sb([128, 128], F32, f"bd{grp}")
    u = S.unit(f"bd{grp}")
    S.op("gpsimd", lambda e: e.memset(t[:], 0.0), w=[u])
    for g in range(128 // grp):
        S.op("gpsimd", lambda e, g=g: e.memset(t[g * grp:(g + 1) * grp, g * grp:(g + 1) * grp], 1.0), w=[u])
    return t, u


def compute_mod(S, k, c_d, wada_d, bada_d, chunk_ids, bank=0):
    cpt = S.sb([128, 8], F32, "cpt")
    ucpt = S.unit("cpt")
    S.dma("sync", cpt[:], c_d, w=[ucpt])
    cond = S.sb([128, 8], F32, "cond")
    ucond = S.unit("cond")
    S.op("scalar", lambda e: e.activation(out=cond[:], in_=cpt[:], func=AF.Silu), r=[ucpt], w=[ucond])
    bada = S.sb([128, 48], F32, "bada")
    ubada = S.unit("bada")
    S.dma("sync", bada[:], bada_d, w=[ubada])
    modT = S.sb([128, 48], F32, "modT")
    umod = S.unit("modT")
    wb = [S.sb([128, 8, 256], F32, f"wada{i}") for i in range(2)]
    uwb = [S.unit(f"wada{i}") for i in range(2)]
    ps = k.pb[bank]
    ups = k.upb[bank]
    for j in chunk_ids:
        for piece in range(4):
            i = rot(k, "wada", 2)
            c0 = j * 1024 + piece * 256
            S.dma("sync", wb[i][:], wada_d[:, c0:c0 + 256].rearrange("(kt p) c -> p kt c", p=128), w=[uwb[i]])
            for t in range(2):
                col = j * 8 + piece * 2 + t
                for kt in range(8):
                    S.op("tensor", lambda e, i=i, t=t, kt=kt, col=col: e.matmul(
                        ps[:, col:col + 1], lhsT=wb[i][:, kt, t * 128:(t + 1) * 128], rhs=cond[:, kt:kt + 1],
                        start=(kt == 0), stop=(kt == 7)), r=[uwb[i], ucond], w=[ups])
    lo = min(chunk_ids) * 8
    hi = (max(chunk_ids) + 1) * 8
    S.op("vector", lambda e: e.tensor_tensor(out=modT[:, lo:hi], in0=ps[:, lo:hi], in1=bada[:, lo:hi], op=ALU.add),
         r=[ups, ubada], w=[umod])
    return modT, umod


def ada_coefs(S, k, modT, umod, g_d, sc_chunk, name):
    g = S.sb([128, 8], F32, f"g_{name}")
    ug = S.unit(f"g_{name}")
    S.dma("sync", g[:], g_d, w=[ug])
    Af = S.sb([128, 8], F32, f"Af_{name}")
    uAf = S.unit(f"Af_{name}")
    S.op("vector", lambda e: e.scalar_tensor_tensor(out=Af[:], in0=modT[:, sc_chunk * 8:(sc_chunk + 1) * 8], scalar=1.0,
                                                     in1=g[:], op0=ALU.add, op1=ALU.mult), r=[umod, ug], w=[uAf])
    return Af, uAf


def mk_norm_scratch(S, k, NT):
    k.sq = [S.sb([128, 512], F32, f"sq{i}") for i in range(2)]
    k.usq = [S.unit(f"sq{i}") for i in range(2)]
    k.tmp = [S.sb([128, 512], F32, f"tmp{i}") for i in range(2)]
    k.utmp = [S.unit(f"tmp{i}") for i in range(2)]
    k.rt = [S.sb([128, 512], F32, f"rt{i}") for i in range(2)]
    k.urt = [S.unit(f"rt{i}") for i in range(2)]
    k.rstd = S.sb([128, NT], F32, "rstd")
    k.urstd = [S.unit(f"rstd{c}") for c in range(NT // 512)]


class Act:
    def __init__(self, t, off, units):
        self.t = t
        self.off = off
        self.units = units

    def ap(self, kt, cs):
        return self.t[:, self.off + kt, cs]

    def u(self, kt, ch):
        return self.units[self.off + kt][ch]


def ada_norm(S, k, xA, NT, Af, uAf, modT, umod, sh_chunk, hA, bank=1):
    ps = k.pb[bank]
    ups = k.upb[bank]
    for ch in range(NT // 512):
        cs = slice(ch * 512, (ch + 1) * 512)
        for kt in range(8):
            i = rot(k, "sq", 2)
            S.op("scalar", lambda e, i=i, kt=kt, cs=cs: e.activation(out=k.sq[i][:], in_=xA.ap(kt, cs), func=AF.Square),
                 r=[xA.u(kt, ch)], w=[k.usq[i]])
            S.op("tensor", lambda e, i=i, kt=kt: e.matmul(ps[:], lhsT=k.ones[:], rhs=k.sq[i][:], start=(kt == 0), stop=(kt == 7)),
                 r=[k.usq[i], k.uones], w=[ups])
        j = rot(k, "rt", 2)
        S.op("scalar", lambda e, j=j: e.activation(out=k.rt[j][:], in_=ps[:], func=AF.Sqrt, scale=1.0 / 1024, bias=EPS),
             r=[ups], w=[k.urt[j]])
        S.op("vector", lambda e, j=j, cs=cs: e.reciprocal(out=k.rstd[:, cs], in_=k.rt[j][:]), r=[k.urt[j]], w=[k.urstd[ch]])
        for kt in range(8):
            i = rot(k, "tmp", 2)
            S.op("vector", lambda e, i=i, kt=kt, cs=cs: e.tensor_tensor(out=k.tmp[i][:], in0=xA.ap(kt, cs), in1=k.rstd[:, cs], op=ALU.mult),
                 r=[xA.u(kt, ch), k.urstd[ch]], w=[k.utmp[i]])
            S.op("scalar", lambda e, i=i, kt=kt, cs=cs: e.activation(
                out=hA.ap(kt, cs), in_=k.tmp[i][:], func=AF.Identity, scale=Af[:, kt:kt + 1],
                bias=modT[:, sh_chunk * 8 + kt:sh_chunk * 8 + kt + 1]), r=[k.utmp[i], uAf, umod], w=[hA.u(kt, ch)])


def load_wgrp(S, k, w_d, c0, ncols, nkt=8, name="wg", nbuf=3, width=512):
    key = f"{name}_bufs"
    if not hasattr(k, key):
        setattr(k, key, ([S.sb([128, nkt, width], BF16, f"{name}{i}") for i in range(nbuf)],
                         [S.unit(f"{name}{i}") for i in range(nbuf)]))
    bufs, us = getattr(k, key)
    i = rot(k, name, nbuf)
    S.dma("gpsimd", bufs[i][:, :, 0:ncols], w_d[:, c0:c0 + ncols].rearrange("(kt p) c -> p kt c", p=128), w=[us[i]])
    return bufs[i], us[i]


def proj_fm(S, k, wg, uwg, col0, M, hA, ch, bank, nkt=8, koff=0):
    ps = k.pb[bank]
    ups = k.upb[bank]
    for kt in range(nkt):
        S.op("tensor", lambda e, kt=kt: e.matmul(ps[0:M, :], lhsT=wg[:, kt, col0:col0 + M], rhs=hA.ap(koff + kt, slice(ch * 512, (ch + 1) * 512)),
                                                start=(kt == 0), stop=(kt == nkt - 1)), r=[uwg, hA.u(koff + kt, ch)], w=[ups])
    return ps, ups


def build_A(NT=2048):
    nc = bass.Bass("TRN2", target_bir_lowering=False)
    xT_d = din(nc, "xT", [1024, NT])
    c_d = din(nc, "c_pt", [128, 8])
    wada_d = din(nc, "w_ada", [1024, 6144])
    bada_d = din(nc, "b_ada", [128, 48])
    g_d = din(nc, "g_mix", [128, 8])
    win_d = din(nc, "w_in", [1024, NIN])
    qg_d = din(nc, "qg", [128, 1])
    kg_d = din(nc, "kg", [128, 1])
    qT_o = dout(nc, "qT", [8, 128, NT], BF16)
    kT_o = dout(nc, "kT", [8, 128, NT], BF16)
    v_o = dout(nc, "v_tok", [NT, 1024], BF16)
    mqT_o = dout(nc, "mqT", [1024, NT], F32)
    mkT_o = dout(nc, "mkT", [1024, NT], F32)
    mv_o = dout(nc, "mv_tok", [NT, 1024], BF16)
    gates_o = dout(nc, "gates", [8, NT], F32)
    NCH = NT // 512
    with ExitStack() as es:
        S = Sched(nc, es)
        k = mk_common(S)
        bd, ubd = mk_blockdiag(S, k, 64)
        mk_norm_scratch(S, k, NT)
        xT = S.sb([128, 8, NT], F32, "xT")
        uxT = [[S.unit(f"xT{kt}_{c}") for c in range(NCH)] for kt in range(8)]
        for kt in range(8):
            S.dma("sync", xT[:, kt, :], xT_d[kt * 128:(kt + 1) * 128, :], w=uxT[kt])
        modT, umod = compute_mod(S, k, c_d, wada_d, bada_d, [0, 1], bank=0)
        Af, uAf = ada_coefs(S, k, modT, umod, g_d, 1, "mix")
        hT = S.sb([128, 8, NT], BF16, "hT")
        uhT = [[S.unit(f"hT{kt}_{c}") for c in range(NCH)] for kt in range(8)]
        xA = Act(xT, 0, uxT)
        hA = Act(hT, 0, uhT)
        ada_norm(S, k, xA, NT, Af, uAf, modT, umod, 0, hA, bank=1)
        qg = S.sb([128, 1], F32, "qg")
        kg = S.sb([128, 1], F32, "kg")
        uqg = S.unit("qg")
        ukg = S.unit("kg")
        S.dma("sync", qg[:], qg_d, w=[uqg])
        S.dma("sync", kg[:], kg_d, w=[ukg])
        S.op("vector", lambda e: e.tensor_scalar(out=qg[:], in0=qg[:], scalar1=0.125, scalar2=None, op0=ALU.mult), r=[uqg], w=[uqg])
        raw = [S.sb([128, 512], F32, f"raw{i}") for i in range(2)]
        uraw = [S.unit(f"raw{i}") for i in range(2)]
        rs = [S.sb([128, 512], F32, f"rs{i}") for i in range(2)]
        urs = [S.unit(f"rs{i}") for i in range(2)]
        stq = [S.sb([128, NT], BF16, f"stq{i}") for i in range(2)]
        ustq = [S.unit(f"stq{i}") for i in range(2)]
        st32 = [S.sb([128, NT], F32, f"st32{i}") for i in range(2)]
        ust32 = [S.unit(f"st32{i}") for i in range(2)]
        sttok = [S.sb([128, 512], BF16, f"sttok{i}") for i in range(3)]
        usttok = [S.unit(f"sttok{i}") for i in range(3)]
        stg = S.sb([8, NT], F32, "stg")
        ustg = S.unit("stg")
        PB = [3, 4, 5, 6, 7]
        uo_stq = [S.unit(f"o_stq{i}") for i in range(2)]
        uo_st32 = [S.unit(f"o_st32{i}") for i in range(2)]
        uo_tok = [S.unit(f"o_tok{i}") for i in range(3)]
        uo_g = S.unit("o_g")
        for which, (c0, gvec, ug, o_d) in enumerate([(0, qg, uqg, qT_o), (1024, kg, ukg, kT_o)]):
            for grp in range(2):
                wg, uwg = load_wgrp(S, k, win_d, c0 + grp * 512, 512)
                for t in range(4):
                    head = grp * 4 + t
                    si = rot(k, "stq", 2)
                    for ch in range(NCH):
                        cs = slice(ch * 512, (ch + 1) * 512)
                        bank = PB[rot(k, "pb", 5)]
                        ps, ups = proj_fm(S, k, wg, uwg, t * 128, 128, hA, ch, bank)
                        ri = rot(k, "raw", 2)
                        qi = rot(k, "sq", 2)
                        S.op("scalar", lambda e, ri=ri, ps=ps: e.activation(out=raw[ri][:], in_=ps[:], func=AF.Copy), r=[ups], w=[uraw[ri]])
                        S.op("scalar", lambda e, qi=qi, ps=ps: e.activation(out=k.sq[qi][:], in_=ps[:], func=AF.Square), r=[ups], w=[k.usq[qi]])
                        S.op("tensor", lambda e, qi=qi: e.matmul(k.pb[2][:], lhsT=bd[:], rhs=k.sq[qi][:], start=True, stop=True),
                             r=[k.usq[qi], ubd], w=[k.upb[2]])
                        ti = rot(k, "rt", 2)
                        S.op("scalar", lambda e, ti=ti: e.activation(out=k.rt[ti][:], in_=k.pb[2][:], func=AF.Sqrt, scale=1.0 / 64, bias=EPS),
                             r=[k.upb[2]], w=[k.urt[ti]])
                        rj = rot(k, "rs", 2)
                        S.op("vector", lambda e, ti=ti, rj=rj: e.reciprocal(out=rs[rj][:], in_=k.rt[ti][:]), r=[k.urt[ti]], w=[urs[rj]])
                        S.op("vector", lambda e, ri=ri, rj=rj, si=si, cs=cs, gvec=gvec: e.scalar_tensor_tensor(
                            out=stq[si][:, cs], in0=raw[ri][:], scalar=gvec[:, 0:1], in1=rs[rj][:], op0=ALU.mult, op1=ALU.mult),
                             r=[uraw[ri], urs[rj], ug], w=[ustq[si]])
                    S.dma("sync", o_d[head], stq[si][:], r=[ustq[si]], w=[uo_stq[si]])
        for which, (c0, o_d) in enumerate([(3072, mqT_o), (4096, mkT_o)]):
            for grp in range(2):
                wg, uwg = load_wgrp(S, k, win_d, c0 + grp * 512, 512)
                for t in range(4):
                    ft = grp * 4 + t
                    si = rot(k, "st32", 2)
                    for ch in range(NCH):
                        cs = slice(ch * 512, (ch + 1) * 512)
                        bank = PB[rot(k, "pb", 5)]
                        ps, ups = proj_fm(S, k, wg, uwg, t * 128, 128, hA, ch, bank)
                        if ch % 2 == 0:
                            S.op("scalar", lambda e, si=si, cs=cs, ps=ps: e.activation(out=st32[si][:, cs], in_=ps[:], func=AF.Copy),
                                 r=[ups], w=[ust32[si]])
                        else:
                            S.op("vector", lambda e, si=si, cs=cs, ps=ps: e.tensor_copy(out=st32[si][:, cs], in_=ps[:]),
                                 r=[ups], w=[ust32[si]])
                    S.dma("sync", o_d[ft * 128:(ft + 1) * 128, :], st32[si][:], r=[ust32[si]], w=[uo_st32[si]])
        wg, uwg = load_wgrp(S, k, win_d, 7168, 8)
        for ch in range(NCH):
            cs = slice(ch * 512, (ch + 1) * 512)
            bank = PB[rot(k, "pb", 5)]
            ps, ups = proj_fm(S, k, wg, uwg, 0, 8, hA, ch, bank)
            S.op("vector", lambda e, cs=cs, ps=ps: e.tensor_copy(out=stg[:, cs], in_=ps[0:8, :]), r=[ups], w=[ustg])
        S.dma("sync", gates_o, stg[:], r=[ustg], w=[uo_g])
        for which, (c0, o_d) in enumerate([(2048, v_o), (5120, mv_o)]):
            for grp in range(2):
                wg, uwg = load_wgrp(S, k, win_d, c0 + grp * 512, 512)
                for tt in range(NT // 128):
                    bank = PB[rot(k, "pb", 5)]
                    ps = k.pb[bank]
                    ups = k.upb[bank]
                    ch = tt // 4
                    for kt in range(8):
                        S.op("tensor", lambda e, kt=kt, tt=tt, ps=ps, wg=wg: e.matmul(
                            ps[:], lhsT=hT[:, kt, tt * 128:(tt + 1) * 128], rhs=wg[:, kt, :], start=(kt == 0), stop=(kt == 7)),
                             r=[uwg, uhT[kt][ch]], w=[ups])
                    si = rot(k, "sttok", 3)
                    if tt % 2 == 0:
                        S.op("scalar", lambda e, si=si, ps=ps: e.activation(out=sttok[si][:], in_=ps[:], func=AF.Copy), r=[ups], w=[usttok[si]])
                    else:
                        S.op("vector", lambda e, si=si, ps=ps: e.tensor_copy(out=sttok[si][:], in_=ps[:]), r=[ups], w=[usttok[si]])
                    S.dma("sync", o_d[tt * 128:(tt + 1) * 128, grp * 512:(grp + 1) * 512], sttok[si][:], r=[usttok[si]], w=[uo_tok[si]])
        S.finish(uo_stq + uo_st32 + uo_tok + [uo_g])
        S.emit()
    return nc


def build_B1(SEQ=16384):
    nc = bass.Bass("TRN2", target_bir_lowering=False)
    qT_d = din(nc, "qT", [128, SEQ], BF16)
    kT_d = din(nc, "kT", [128, SEQ], BF16)
    v_d = din(nc, "v", [SEQ, 128], BF16)
    T0b_d = din(nc, "T0b", [128, 128])
    M0_d = din(nc, "M0", [128, 128])
    T1_d = din(nc, "T1", [128, 128])
    b31_d = din(nc, "b31", [128, 1])
    sm_d = din(nc, "small", [1, 512])
    subg_d = din(nc, "subg", [128, 1])
    ya_o = dout(nc, "yaT", [128, SEQ], BF16)
    NQT = SEQ // 512
    NKB = SEQ // 128
    with ExitStack() as es:
        S = Sched(nc, es)
        k = mk_common(S)
        ones_b = S.sb([128, 128], BF16, "ones_b")
        uones_b = S.unit("ones_b")
        S.op("gpsimd", lambda e: e.memset(ones_b[:], 1.0), w=[uones_b])
        qT = S.sb([128, SEQ], BF16, "qT")
        kT = S.sb([128, SEQ], BF16, "kT")
        vv = S.sb([128, NKB, 128], BF16, "vv")
        NP = max(1, SEQ // 2048)
        PW = SEQ // NP
        uq = [S.unit(f"q{i}") for i in range(NP)]
        uk = [S.unit(f"k{i}") for i in range(NP)]
        uv = [S.unit(f"v{i}") for i in range(NP)]
        v_r = v_d.rearrange("(kb p) d -> p kb d", p=128)
        for i in range(NP):
            S.dma("sync", qT[:, i * PW:(i + 1) * PW], qT_d[:, i * PW:(i + 1) * PW], w=[uq[i]])
            S.dma("sync", kT[:, i * PW:(i + 1) * PW], kT_d[:, i * PW:(i + 1) * PW], w=[uk[i]])
            S.dma("gpsimd", vv[:, i * (PW // 128):(i + 1) * (PW // 128), :], v_r[:, i * (PW // 128):(i + 1) * (PW // 128), :], w=[uv[i]])
        sm = S.sb([1, 512], F32, "sm")
        usm = S.unit("sm")
        S.dma("sync", sm[:], sm_d, w=[usm])
        sc = S.sb([1, 512], F32, "sc")
        usc = S.unit("sc")
        vals = S.sb([1, 8], F32, "vals")
        uvals = S.unit("vals")
        S.op("scalar", lambda e: e.activation(out=sc[:, 0:128], in_=sm[:, 0:128], func=AF.Abs), r=[usm], w=[usc])
        S.op("vector", lambda e: e.tensor_reduce(out=sc[:, 128:130], in_=sc[:, 0:128].rearrange("p (a b) -> p a b", a=2), axis=AX.X, op=ALU.max),
             r=[usc], w=[usc])
        S.op("vector", lambda e: e.tensor_tensor(out=sc[:, 130:131], in0=sc[:, 128:129], in1=sc[:, 129:130], op=ALU.mult), r=[usc], w=[usc])
        S.op("vector", lambda e: e.tensor_scalar(out=vals[:, 0:1], in0=sc[:, 130:131], scalar1=-8.0, scalar2=None, op0=ALU.mult), r=[usc], w=[uvals])
        S.op("vector", lambda e: e.tensor_tensor(out=sc[:, 256:384].rearrange("p (a b) -> p a b", a=2),
                                                 in0=sm[:, 128:384].rearrange("p (a t b) -> p a t b", a=2, t=2)[:, :, 0, :],
                                                 in1=sm[:, 128:384].rearrange("p (a t b) -> p a t b", a=2, t=2)[:, :, 1, :], op=ALU.mult),
             r=[usm, usc], w=[usc])
        S.op("vector", lambda e: e.tensor_reduce(out=sc[:, 384:386], in_=sc[:, 256:384].rearrange("p (a b) -> p a b", a=2), axis=AX.X, op=ALU.add),
             r=[usc], w=[usc])
        S.op("scalar", lambda e: e.activation(out=sc[:, 386:388], in_=sc[:, 384:386], func=AF.Exp), r=[usc], w=[usc])
        S.op("vector", lambda e: e.tensor_tensor(out=sc[:, 388:389], in0=sc[:, 387:388], in1=sc[:, 386:387], op=ALU.subtract), r=[usc], w=[usc])
        S.op("vector", lambda e: e.tensor_tensor(out=vals[:, 1:2], in0=sc[:, 388:389], in1=sm[:, 384:385], op=ALU.subtract), r=[usc, usm], w=[uvals])
        S.op("vector", lambda e: e.tensor_scalar(out=vals[:, 2:3], in0=sm[:, 384:385], scalar1=-1.0, scalar2=1.0, op0=ALU.mult, op1=ALU.add),
             r=[usm], w=[uvals])
        bc = S.sb([128, 8], F32, "bc")
        ubc = S.unit("bc")
        S.op("tensor", lambda e: e.matmul(k.pb[0][:, 0:4], lhsT=k.ones[0:1, :], rhs=vals[:, 0:4], start=True, stop=True), r=[uvals, k.uones], w=[k.upb[0]])
        S.op("vector", lambda e: e.tensor_copy(out=bc[:, 0:4], in_=k.pb[0][:, 0:4]), r=[k.upb[0]], w=[ubc])
        T0 = S.sb([128, 128], F32, "T0")
        M0 = S.sb([128, 128], F32, "M0")
        T1 = S.sb([128, 128], F32, "T1")
        b31 = S.sb([128, 1], F32, "b31")
        subg = S.sb([128, 1], F32, "subg")
        uT = S.unit("Tconst")
        uT2 = S.unit("Tconst2")
        S.dma("sync", T0[:], T0b_d, w=[uT])
        S.dma("sync", M0[:], M0_d, w=[uT])
        S.dma("sync", T1[:], T1_d, w=[uT])
        S.dma("sync", b31[:], b31_d, w=[uT])
        S.dma("sync", subg[:], subg_d, w=[uT])
        cb = S.sb([128, 1], F32, "cb")
        S.op("vector", lambda e: e.tensor_tensor(out=cb[:], in0=b31[:], in1=bc[:, 0:1], op=ALU.add), r=[uT, ubc], w=[uT2])
        S.op("vector", lambda e: e.scalar_tensor_tensor(out=T0[:], in0=T0[:], scalar=bc[:, 0:1], in1=M0[:], op0=ALU.add, op1=ALU.add), r=[uT, ubc], w=[uT2])
        S.op("vector", lambda e: e.tensor_scalar(out=T1[:], in0=T1[:], scalar1=bc[:, 0:1], scalar2=None, op0=ALU.add), r=[uT, ubc], w=[uT2])
        S.op("vector", lambda e: e.tensor_scalar(out=subg[:], in0=subg[:], scalar1=bc[:, 2:3], scalar2=None, op0=ALU.mult), r=[uT, ubc], w=[uT2])
        BT = S.sb([128, 5, 512], F32, "BT")
        uBT = S.unit("BT")
        S.op("gpsimd", lambda e: e.memset(BT[:], -30000.0), w=[uBT])
        for ii in range(5):
            i = ii - 1
            for j in range(4):
                js = slice(j * 128, (j + 1) * 128)
                if j == i:
                    S.op("vector", lambda e, ii=ii, js=js: e.tensor_copy(out=BT[:, ii, js], in_=T0[:]), r=[uT2], w=[uBT])
                elif j == i + 1:
                    S.op("vector", lambda e, ii=ii, js=js: e.tensor_copy(out=BT[:, ii, js], in_=T1[:]), r=[uT2], w=[uBT])
                elif j > i + 1:
                    S.op("vector", lambda e, ii=ii, js=js: e.tensor_scalar(out=BT[:, ii, js], in0=M0[:], scalar1=0.0, scalar2=cb[:, 0:1],
                                                                        op0=ALU.mult, op1=ALU.add), r=[uT2, uT], w=[uBT])
        NE = 3
        Eb = [[S.sb([128, 512], BF16, f"E{n}_{c}") for c in range(2)] for n in range(NE)]
        uE = [[S.unit(f"E{n}_{c}") for c in range(2)] for n in range(NE)]
        tmpf = [S.sb([128, 512], F32, f"tf{n}") for n in range(2)]
        utmpf = [S.unit(f"tf{n}") for n in range(2)]
        r1 = S.sb([128, 2, 512], F32, "r1")
        ur1 = S.unit("r1")
        y = S.sb([128, 2, 512], F32, "y")
        uy = S.unit("y")
        sq = S.sb([128, 512], F32, "sqy")
        usq = S.unit("sqy")
        rt = S.sb([128, 512], F32, "rty")
        urt = S.unit("rty")
        stage = [S.sb([128, 512], BF16, f"stage{n}") for n in range(2)]
        ustage = [S.unit(f"stage{n}") for n in range(2)]
        uo = [S.unit(f"o{n}") for n in range(2)]
        units = [(qt, kb) for qt in range(NQT) for kb in range(4 * qt + 4)]

        def piece(col):
            return min(col // PW, NP - 1)

        def emit_S(idx):
            qt, kb = units[idx]
            sp = idx % 2
            qs = slice(qt * 512, (qt + 1) * 512)
            ks = slice(kb * 128, (kb + 1) * 128)
            n = idx % NE
            for c in range(2):
                bank = 2 * sp + c
                rows = slice(64 * c, 64 * (c + 1))
                S.op("tensor", lambda e, bank=bank, rows=rows, ks=ks, qs=qs: e.matmul(
                    k.pb[bank][:], lhsT=kT[rows, ks], rhs=qT[rows, qs], start=True, stop=True),
                     r=[uk[piece(kb * 128)], uq[piece(qt * 512)]], w=[k.upb[bank]])
            i = kb - 4 * qt
            for c in range(2):
                bank = 2 * sp + c
                if i < -1:
                    S.op("scalar", lambda e, bank=bank, n=n, c=c: e.activation(out=Eb[n][c][:], in_=k.pb[bank][:], func=AF.Exp, bias=cb[:, 0:1], scale=1.0),
                         r=[k.upb[bank], uT2], w=[uE[n][c]])
                else:
                    ti = rot(k, "tf", 2)
                    S.op("vector", lambda e, bank=bank, ti=ti, i=i: e.tensor_tensor(out=tmpf[ti][:], in0=k.pb[bank][:], in1=BT[:, i + 1, :], op=ALU.add),
                         r=[k.upb[bank], uBT], w=[utmpf[ti]])
                    S.op("scalar", lambda e, ti=ti, n=n, c=c: e.activation(out=Eb[n][c][:], in_=tmpf[ti][:], func=AF.Exp),
                         r=[utmpf[ti]], w=[uE[n][c]])

        def emit_PV(idx):
            qt, kb = units[idx]
            n = idx % NE
            last = (kb == 4 * qt + 3)
            for c in range(2):
                S.op("tensor", lambda e, c=c, kb=kb, n=n, last=last: e.matmul(
                    k.pb[4 + c][:], lhsT=vv[:, kb, :], rhs=Eb[n][c][:], start=(kb == 0), stop=last),
                     r=[uv[piece(kb * 128)], uE[n][c]], w=[k.upb[4 + c]])
            for c in range(2):
                S.op("tensor", lambda e, c=c, kb=kb, n=n, last=last: e.matmul(
                    k.pb[6 + c][:], lhsT=ones_b[:], rhs=Eb[n][c][:], start=(kb == 0), stop=last),
                     r=[uones_b, uE[n][c]], w=[k.upb[6 + c]])
            if last:
                emit_fin(qt)

        def emit_fin(qt):
            qs = slice(qt * 512, (qt + 1) * 512)
            for c in range(2):
                S.op("vector", lambda e, c=c: e.reciprocal(out=r1[:, c, :], in_=k.pb[6 + c][:]), r=[k.upb[6 + c]], w=[ur1])
            for c in range(2):
                S.op("vector", lambda e, c=c: e.tensor_tensor(out=y[:, c, :], in0=k.pb[4 + c][:], in1=r1[:, c, :], op=ALU.mult),
                     r=[k.upb[4 + c], ur1], w=[uy])
            S.op("vector", lambda e: e.scalar_tensor_tensor(out=y[:, 0, :], in0=y[:, 1, :], scalar=bc[:, 1:2], in1=y[:, 0, :],
                                                             op0=ALU.mult, op1=ALU.add), r=[uy, ubc], w=[uy])
            S.op("scalar", lambda e: e.activation(out=sq[:], in_=y[:, 0, :], func=AF.Square), r=[uy], w=[usq])
            S.op("tensor", lambda e: e.matmul(k.pb[0][:], lhsT=k.ones[:], rhs=sq[:], start=True, stop=True), r=[usq, k.uones], w=[k.upb[0]])
            S.op("scalar", lambda e: e.activation(out=rt[:], in_=k.pb[0][:], func=AF.Sqrt, scale=1.0 / 128, bias=EPS), r=[k.upb[0]], w=[urt])
            S.op("vector", lambda e: e.reciprocal(out=rt[:], in_=rt[:]), r=[urt], w=[urt])
            si = qt % 2
            S.op("vector", lambda e, si=si: e.scalar_tensor_tensor(out=stage[si][:], in0=y[:, 0, :], scalar=subg[:, 0:1], in1=rt[:],
                                                                   op0=ALU.mult, op1=ALU.mult), r=[uy, urt, uT2], w=[ustage[si]])
            S.dma("sync", ya_o[:, qs], stage[si][:], r=[ustage[si]], w=[uo[si]])

        emit_S(0)
        for idx in range(len(units)):
            if idx + 1 < len(units):
                emit_S(idx + 1)
            emit_PV(idx)
        S.finish(uo)
        S.emit()
    return nc


def build_B2(SEQ=16384):
    nc = bass.Bass("TRN2", target_bir_lowering=False)
    NC = SEQ // 64
    SC = 1024
    NSC = SEQ // SC
    CPS = SC // 64
    mqT_d = din(nc, "mqT", [256, SEQ])
    mkT_d = din(nc, "mkT", [256, SEQ])
    cw_d = din(nc, "cw", [128, 2, 2, 4])
    cbias_d = din(nc, "cbias", [128, 2, 2])
    mv_d = din(nc, "mv", [SEQ, 128], BF16)
    gi_d = din(nc, "gi", [64, NC])
    gf_d = din(nc, "gf", [64, NC])
    gb_d = din(nc, "gb", [64, 2])
    utri_d = din(nc, "utri", [64, 64])
    ident_d = din(nc, "ident", [128, 128])
    h_o = dout(nc, "h", [SEQ, 128], F32)
    with ExitStack() as es:
        S = Sched(nc, es)
        k = K()
        k.rr = {}
        ones = S.sb([128, 128], F32, "ones")
        uones = S.unit("ones")
        S.op("gpsimd", lambda e: e.memset(ones[:], 1.0), w=[uones])
        pf = [S.ps([128, 512], F32, f"pf{i}") for i in range(6)]
        upf = [S.unit(f"pf{i}") for i in range(6)]
        pbf = [S.ps([128, 1024], BF16, f"pbf{i}") for i in range(2)]
        upbf = [S.unit(f"pbf{i}") for i in range(2)]
        utri = S.sb([64, 64], F32, "utri")
        ident = S.sb([128, 128], F32, "ident")
        identb = S.sb([128, 128], BF16, "identb")
        maskb = S.sb([64, 64], F32, "maskb")
        ucst = S.unit("cst")
        ucst2 = S.unit("cst2")
        S.dma("sync", utri[:], utri_d, w=[ucst])
        S.dma("sync", ident[:], ident_d, w=[ucst])
        S.op("vector", lambda e: e.tensor_copy(out=identb[:], in_=ident[:]), r=[ucst], w=[ucst2])
        cw = S.sb([128, 2, 2, 4], F32, "cw")
        cbias = S.sb([128, 2, 2], F32, "cbias")
        S.dma("sync", cw[:], cw_d, w=[ucst])
        S.dma("sync", cbias[:], cbias_d, w=[ucst])
        gi = S.sb([64, NC], F32, "gi")
        gf = S.sb([64, NC], F32, "gf")
        gb = S.sb([64, 2], F32, "gb")
        ug = S.unit("g")
        S.dma("sync", gi[:], gi_d, w=[ug])
        S.dma("sync", gf[:], gf_d, w=[ug])
        S.dma("sync", gb[:], gb_d, w=[ug])
        lnv = S.sb([64, NC], F32, "lnv")
        ulnv = S.unit("lnv")
        S.op("vector", lambda e: e.tensor_scalar(out=lnv[:], in0=gf[:], scalar1=gb[:, 1:2], scalar2=-1.0, op0=ALU.add, op1=ALU.mult), r=[ug], w=[ulnv])
        S.op("scalar", lambda e: e.activation(out=lnv[:], in_=lnv[:], func=AF.Exp), r=[ulnv], w=[ulnv])
        S.op("scalar", lambda e: e.activation(out=lnv[:], in_=lnv[:], func=AF.Ln, bias=1.0, scale=1.0), r=[ulnv], w=[ulnv])
        NH = (NC + 511) // 512
        CW = min(NC, 512)
        nb = S.sb([64, NC], F32, "nb")
        unb = S.unit("nb")
        vv = S.sb([64, NC], F32, "vvg")
        uvv = S.unit("vvg")
        for hh in range(NH):
            cs = slice(hh * CW, (hh + 1) * CW)
            S.op("tensor", lambda e, cs=cs: e.matmul(pf[0][0:64, 0:CW], lhsT=utri[:], rhs=lnv[:, cs], start=True, stop=True), r=[ucst, ulnv], w=[upf[0]])
            S.op("vector", lambda e, cs=cs: e.tensor_copy(out=nb[:, cs], in_=pf[0][0:64, 0:CW]), r=[upf[0]], w=[unb])
        S.op("vector", lambda e: e.scalar_tensor_tensor(out=vv[:], in0=gi[:], scalar=gb[:, 0:1], in1=nb[:], op0=ALU.add, op1=ALU.add), r=[ug, unb], w=[uvv])
        NT128 = (NC + 127) // 128
        TW = min(NC, 128)
        umax = S.sb([128, NT128], F32, "umax")
        uumax = S.unit("umax")
        for t in range(NT128):
            S.op("tensor", lambda e, t=t: e.transpose(out=pf[1][0:TW, 0:64], in_=vv[:, t * TW:(t + 1) * TW], identity=ident[0:64, 0:64]),
                 r=[uvv, ucst], w=[upf[1]])
            S.op("vector", lambda e, t=t: e.tensor_reduce(out=umax[0:TW, t:t + 1], in_=pf[1][0:TW, 0:64], axis=AX.X, op=ALU.max), r=[upf[1]], w=[uumax])
        rows = S.sb([1, 4, NC], F32, "rows")
        urows = S.unit("rows")
        for t in range(NT128):
            S.op("tensor", lambda e, t=t: e.transpose(out=pf[2][0:1, t * TW:(t + 1) * TW], in_=umax[0:TW, t:t + 1], identity=ident[0:TW, 0:TW]),
                 r=[uumax, ucst], w=[upf[2]])
        S.op("vector", lambda e: e.tensor_copy(out=rows[:, 0, :], in_=pf[2][0:1, 0:NC]), r=[upf[2]], w=[urows])
        for hh in range(NH):
            cs = slice(hh * CW, (hh + 1) * CW)
            S.op("tensor", lambda e, cs=cs: e.matmul(pf[3][0:1, 0:CW], lhsT=ident[0:64, 63:64], rhs=nb[:, cs], start=True, stop=True), r=[ucst, unb], w=[upf[3]])
            S.op("vector", lambda e, cs=cs: e.tensor_scalar(out=rows[:, 1, cs], in0=pf[3][0:1, 0:CW], scalar1=-1.0, scalar2=None, op0=ALU.mult), r=[upf[3]], w=[urows])
        S.op("vector", lambda e: e.tensor_tensor_scan(out=rows[:, 2, :], data0=rows[:, 0, :], data1=rows[:, 1, :], initial=0.0, op0=ALU.max, op1=ALU.add),
             r=[urows], w=[urows])
        R2 = S.sb([1, 2, NC], F32, "R2")
        uR2 = S.unit("R2")
        S.op("vector", lambda e: e.tensor_tensor(out=R2[:, 0, :], in0=rows[:, 2, :], in1=rows[:, 1, :], op=ALU.subtract), r=[urows], w=[uR2])
        S.op("vector", lambda e: e.memset(rows[:, 3, 0:1], 0.0), r=[urows], w=[urows])
        if NC > 1:
            S.op("vector", lambda e: e.tensor_copy(out=rows[:, 3, 1:NC], in_=rows[:, 2, 0:NC - 1]), r=[urows], w=[urows])
        S.op("vector", lambda e: e.tensor_tensor(out=rows[:, 3, :], in0=rows[:, 3, :], in1=R2[:, 0, :], op=ALU.subtract), r=[urows, uR2], w=[urows])
        S.op("scalar", lambda e: e.activation(out=R2[:, 1, :], in_=rows[:, 3, :], func=AF.Exp), r=[urows], w=[uR2])
        Mcb = S.sb([128, NC], F32, "Mcb")
        ab = S.sb([128, NC], F32, "ab")
        ubc = S.unit("bc")
        for hh in range(NH):
            cs = slice(hh * CW, (hh + 1) * CW)
            S.op("tensor", lambda e, cs=cs: e.matmul(pf[4][:, 0:CW], lhsT=ones[0:1, :], rhs=R2[:, 0, cs], start=True, stop=True), r=[uR2, uones], w=[upf[4]])
            S.op("vector", lambda e, cs=cs: e.tensor_copy(out=Mcb[:, cs], in_=pf[4][:, 0:CW]), r=[upf[4]], w=[ubc])
            S.op("tensor", lambda e, cs=cs: e.matmul(pf[5][:, 0:CW], lhsT=ones[0:1, :], rhs=R2[:, 1, cs], start=True, stop=True), r=[uR2, uones], w=[upf[5]])
            S.op("vector", lambda e, cs=cs: e.tensor_copy(out=ab[:, cs], in_=pf[5][:, 0:CW]), r=[upf[5]], w=[ubc])
        wgt = S.sb([64, NC], F32, "wgt")
        epsd = S.sb([64, NC], F32, "epsd")
        uw = S.unit("wgt")
        S.op("vector", lambda e: e.tensor_tensor(out=wgt[:], in0=vv[:], in1=Mcb[0:64, :], op=ALU.subtract), r=[uvv, ubc], w=[uw])
        S.op("scalar", lambda e: e.activation(out=wgt[:], in_=wgt[:], func=AF.Exp), r=[uw], w=[uw])
        S.op("vector", lambda e: e.tensor_scalar(out=wgt[:], in0=wgt[:], scalar1=0.0625, scalar2=None, op0=ALU.mult), r=[uw], w=[uw])
        S.op("vector", lambda e: e.tensor_tensor(out=epsd[:], in0=nb[:], in1=Mcb[0:64, :], op=ALU.subtract), r=[unb, ubc], w=[uw])
        S.op("scalar", lambda e: e.activation(out=epsd[:], in_=epsd[:], func=AF.Exp), r=[uw], w=[uw])
        C32 = S.sb([128, 2, 130], F32, "C32")
        Cb = S.sb([128, 2, 130], BF16, "Cb")
        uC32 = S.unit("C32")
        uCb = S.unit("Cb")
        S.op("vector", lambda e: e.memset(C32[:], 0.0), w=[uC32])
        S.op("vector", lambda e: e.memset(Cb[:], 0.0), w=[uCb])
        stg = [[S.sb([128, 2, SC + 3], F32, f"stg{b}_{w}") for w in range(2)] for b in range(2)]
        ustg = [[S.unit(f"stg{b}_{w}") for w in range(2)] for b in range(2)]
        acc = [S.sb([128, 2, SC], F32, f"acc{w}") for w in range(2)]
        uacc = [S.unit(f"acc{w}") for w in range(2)]
        QK = [[S.sb([128, 2, SC], BF16, f"QK{b}_{w}") for w in range(2)] for b in range(2)]
        uQK = [[S.unit(f"QK{b}_{w}") for w in range(2)] for b in range(2)]
        Vp = [S.sb([64, CPS, 130], BF16, f"Vp{b}") for b in range(2)]
        uVp = [S.unit(f"Vp{b}") for b in range(2)]
        hst = [S.sb([64, CPS, 128], F32, f"hst{b}") for b in range(2)]
        uhst = [S.unit(f"hst{b}") for b in range(2)]
        uho = [S.unit(f"ho{b}") for b in range(2)]
        Sm = [S.sb([64, 64], BF16, f"Sm{i}") for i in range(2)]
        uSm = [S.unit(f"Sm{i}") for i in range(2)]
        Kt = [S.sb([64, 256], BF16, f"Kt{i}") for i in range(2)]
        uKt = [S.unit(f"Kt{i}") for i in range(2)]
        VW = [S.sb([64, 130], BF16, f"VW{i}") for i in range(2)]
        uVW = [S.unit(f"VW{i}") for i in range(2)]
        dn = [S.sb([64, 2], F32, f"dn{i}") for i in range(2)]
        udn = [S.unit(f"dn{i}") for i in range(2)]
        mv_r = mv_d.rearrange("(c s) e -> s c e", s=64)
        h_r = h_o.rearrange("(c s) e -> s c e", s=64)
        srcs = [mqT_d, mkT_d]

        def prep(sc):
            b = sc % 2
            s0 = sc * SC
            for w in range(2):
                st = stg[b][w]
                if sc == 0:
                    S.op("gpsimd", lambda e, st=st: e.memset(st[:, :, 0:3], 0.0), w=[ustg[b][w]])
                    for dt in range(2):
                        S.dma("sync", st[:, dt, 3:SC + 3], srcs[w][dt * 128:(dt + 1) * 128, 0:SC], w=[ustg[b][w]])
                else:
                    for dt in range(2):
                        S.dma("sync", st[:, dt, :], srcs[w][dt * 128:(dt + 1) * 128, s0 - 3:s0 + SC], w=[ustg[b][w]])
                for dt in range(2):
                    S.op("vector", lambda e, st=st, w=w, dt=dt: e.tensor_scalar(
                        out=acc[w][:, dt, :], in0=st[:, dt, 3:SC + 3], scalar1=cw[:, w, dt, 3:4], scalar2=cbias[:, w, dt:dt + 1],
                        op0=ALU.mult, op1=ALU.add), r=[ustg[b][w], ucst], w=[uacc[w]])
                    for j in range(3):
                        S.op("vector", lambda e, st=st, w=w, dt=dt, j=j: e.scalar_tensor_tensor(
                            out=acc[w][:, dt, :], in0=st[:, dt, j:SC + j], scalar=cw[:, w, dt, j:j + 1], in1=acc[w][:, dt, :],
                            op0=ALU.mult, op1=ALU.add), r=[ustg[b][w], ucst, uacc[w]], w=[uacc[w]])
                S.op("scalar", lambda e, w=w, b=b: e.activation(out=QK[b][w][:], in_=acc[w][:], func=AF.Silu), r=[uacc[w]], w=[uQK[b][w]])
            S.op("gpsimd", lambda e, b=b: e.memset(Vp[b][:, :, 128:130], 1.0), w=[uVp[b]])
            S.dma("gpsimd", Vp[b][:, :, 0:128], mv_r[:, sc * CPS:(sc + 1) * CPS, :], w=[uVp[b]])

        def chunk(c):
            sc = c // CPS
            b = sc % 2
            cc = c % CPS
            cs = slice(cc * 64, (cc + 1) * 64)
            QT = QK[b][0]
            KT = QK[b][1]
            i2 = c % 2
            ps_s = pf[i2]
            ps_h = pf[2 + i2]
            ps_c = pf[4 + i2]
            ps_k = pbf[i2]
            for dt in range(2):
                S.op("tensor", lambda e, dt=dt: e.matmul(ps_s[0:64, 0:64], lhsT=KT[:, dt, cs], rhs=QT[:, dt, cs], start=(dt == 0), stop=(dt == 1)),
                     r=[uQK[b][0], uQK[b][1]], w=[upf[i2]])
            for dt in range(2):
                S.op("tensor", lambda e, dt=dt: e.transpose(out=ps_k[0:64, dt * 128:(dt + 1) * 128], in_=KT[:, dt, cs], identity=identb[:]),
                     r=[uQK[b][1], ucst2], w=[upbf[i2]])
            S.op("vector", lambda e: e.scalar_tensor_tensor(out=Sm[i2][:], in0=ps_s[0:64, 0:64], scalar=wgt[:, c:c + 1], in1=utri[:],
                                                             op0=ALU.mult, op1=ALU.mult), r=[upf[i2], uw, ucst], w=[uSm[i2]])
            S.op("scalar", lambda e: e.activation(out=Kt[i2][:], in_=ps_k[0:64, 0:256], func=AF.Copy), r=[upbf[i2]], w=[uKt[i2]])
            S.op("gpsimd", lambda e: e.tensor_scalar(out=VW[i2][:], in0=Vp[b][:, cc, :], scalar1=wgt[:, c:c + 1], scalar2=None, op0=ALU.mult),
                 r=[uVp[b], uw], w=[uVW[i2]])
            S.op("tensor", lambda e: e.matmul(ps_h[0:64, 0:129], lhsT=Sm[i2][:], rhs=Vp[b][:, cc, 0:129], start=True, stop=False),
                 r=[uSm[i2], uVp[b]], w=[upf[2 + i2]])
            for dt in range(2):
                S.op("tensor", lambda e, dt=dt: e.matmul(ps_h[0:64, 0:129], lhsT=QT[:, dt, cs], rhs=Cb[:, dt, 0:129], start=False, stop=(dt == 1)),
                     r=[uQK[b][0], uCb], w=[upf[2 + i2]])
            for dt in range(2):
                S.op("tensor", lambda e, dt=dt: e.matmul(ps_c[:, dt * 130:dt * 130 + 129], lhsT=Kt[i2][:, dt * 128:(dt + 1) * 128], rhs=VW[i2][:, 0:129],
                                                        start=True, stop=True), r=[uKt[i2], uVW[i2]], w=[upf[4 + i2]])
            S.op("vector", lambda e: e.scalar_tensor_tensor(out=C32[:, :, 0:129], in0=C32[:, :, 0:129], scalar=ab[:, c:c + 1],
                                                             in1=ps_c[:, 0:260].rearrange("p (a b) -> p a b", a=2)[:, :, 0:129],
                                                             op0=ALU.mult, op1=ALU.add), r=[uC32, ubc, upf[4 + i2]], w=[uC32])
            if c + 1 < NC:
                S.op("scalar", lambda e: e.activation(out=Cb[:, :, 0:129], in_=C32[:, :, 0:129], func=AF.Copy, scale=ab[:, c + 1:c + 2]),
                     r=[uC32, ubc], w=[uCb])
            S.op("scalar", lambda e: e.activation(out=dn[i2][:, 0:1], in_=ps_h[0:64, 128:129], func=AF.Abs), r=[upf[2 + i2]], w=[udn[i2]])
            S.op("vector", lambda e: e.tensor_tensor(out=dn[i2][:, 1:2], in0=dn[i2][:, 0:1], in1=epsd[:, c:c + 1], op=ALU.max), r=[udn[i2], uw], w=[udn[i2]])
            S.op("vector", lambda e: e.reciprocal(out=dn[i2][:, 1:2], in_=dn[i2][:, 1:2]), r=[udn[i2]], w=[udn[i2]])
            S.op("scalar", lambda e: e.activation(out=hst[b][:, cc, :], in_=ps_h[0:64, 0:128], func=AF.Copy, scale=dn[i2][:, 1:2]),
                 r=[upf[2 + i2], udn[i2]], w=[uhst[b]])
            if cc == CPS - 1:
                S.dma("sync", h_r[:, sc * CPS:(sc + 1) * CPS, :], hst[b][:], r=[uhst[b]], w=[uho[b]])

        prep(0)
        for sc in range(NSC):
            if sc + 1 < NSC:
                prep(sc + 1)
            for cc in range(CPS):
                chunk(sc * CPS + cc)
        S.finish(uho)
        S.emit()
    return nc


def build_C(NTOT=2048):
    nc = bass.Bass("TRN2", target_bir_lowering=False)
    NT = 1024
    NH = NTOT // NT
    NCH = NT // 512
    xT_d = din(nc, "xT", [1024, NTOT])
    ya_d = din(nc, "yaT", [1024, NTOT], BF16)
    hm_d = din(nc, "hmT", [1024, NTOT])
    c_d = din(nc, "c_pt", [128, 8])
    wada_d = din(nc, "w_ada", [1024, 6144])
    bada_d = din(nc, "b_ada", [128, 48])
    gmix_d = din(nc, "g_mix", [128, 8])
    gffn_d = din(nc, "g_ffn", [128, 8])
    mhn_d = din(nc, "mhn", [128, 2])
    win_d = din(nc, "w_in", [1024, NIN])
    wa_d = din(nc, "w_a", [1024, 1024])
    wm_d = din(nc, "w_m", [1024, 1024])
    wo_d = din(nc, "w_out", [1024, 1024])
    w1_d = din(nc, "w_ff1", [1024, 4096])
    w2_d = din(nc, "w_ff2", [4096, 1024])
    x_o = dout(nc, "xT_out", [1024, NTOT])
    with ExitStack() as es:
        S = Sched(nc, es)
        k = mk_common(S)
        mk_norm_scratch(S, k, NT)
        modT, umod = compute_mod(S, k, c_d, wada_d, bada_d, [0, 1, 2, 3, 4, 5], bank=0)
        Amix, uAmix = ada_coefs(S, k, modT, umod, gmix_d, 1, "mix")
        Affn, uAffn = ada_coefs(S, k, modT, umod, gffn_d, 4, "ffn")
        mhn = S.sb([128, 2], F32, "mhn")
        umhn = S.unit("mhn")
        S.dma("sync", mhn[:], mhn_d, w=[umhn])
        xT = S.sb([128, 8, NT], F32, "xTs")
        uxT = [[S.unit(f"xT{kt}_{c}") for c in range(NCH)] for kt in range(8)]
        xA = Act(xT, 0, uxT)
        act = S.sb([128, 32, NT], BF16, "act")
        uact = [[S.unit(f"act{sl}_{c}") for c in range(NCH)] for sl in range(32)]
        hA = Act(act, 0, uact)
        ymA = Act(act, 8, uact)
        yaA = Act(act, 16, uact)
        h2A = Act(act, 16, uact)
        mgA = Act(act, 24, uact)
        uA = Act(act, 0, uact)
        hms = S.sb([128, 2, NT], F32, "hms")
        uhms = [S.unit(f"hms{c}") for c in range(NCH)]
        sg = [S.sb([128, 512], F32, f"sg{i}") for i in range(4)]
        usg = [S.unit(f"sg{i}") for i in range(4)]
        uxo = [S.unit(f"xo{kt}") for kt in range(8)]
        PB = [3, 4, 5, 6, 7]

        def nb():
            return PB[rot(k, "pb", 5)]

        for half in range(NH):
            t0 = half * NT
            for kt in range(8):
                S.dma("sync", xT[:, kt, :], xT_d[kt * 128:(kt + 1) * 128, t0:t0 + NT], w=uxT[kt])
            for kt in range(8):
                S.dma("sync", act[:, 16 + kt, :], ya_d[kt * 128:(kt + 1) * 128, t0:t0 + NT], w=uact[16 + kt])
            ada_norm(S, k, xA, NT, Amix, uAmix, modT, umod, 0, hA, bank=1)
            for hh in range(4):
                for dt in range(2):
                    S.dma("sync", hms[:, dt, :], hm_d[hh * 256 + dt * 128: hh * 256 + (dt + 1) * 128, t0:t0 + NT], w=uhms)
                if hh % 2 == 0:
                    wg, uwg = load_wgrp(S, k, win_d, 6144 + (hh // 2) * 512, 512, nbuf=4)
                for ch in range(NCH):
                    cs = slice(ch * 512, (ch + 1) * 512)
                    for dt in range(2):
                        i = rot(k, "sq", 2)
                        S.op("scalar", lambda e, i=i, dt=dt, cs=cs: e.activation(out=k.sq[i][:], in_=hms[:, dt, cs], func=AF.Square),
                             r=[uhms[ch]], w=[k.usq[i]])
                        S.op("tensor", lambda e, i=i, dt=dt: e.matmul(k.pb[2][:], lhsT=k.ones[:], rhs=k.sq[i][:], start=(dt == 0), stop=(dt == 1)),
                             r=[k.usq[i], k.uones], w=[k.upb[2]])
                    j = rot(k, "rt", 2)
                    S.op("scalar", lambda e, j=j: e.activation(out=k.rt[j][:], in_=k.pb[2][:], func=AF.Sqrt, scale=1.0 / 256, bias=EPS),
                         r=[k.upb[2]], w=[k.urt[j]])
                    S.op("vector", lambda e, j=j: e.reciprocal(out=k.rt[j][:], in_=k.rt[j][:]), r=[k.urt[j]], w=[k.urt[j]])
                    for dt in range(2):
                        ft = hh * 2 + dt
                        bank = nb()
                        ps, ups = proj_fm(S, k, wg, uwg, (ft % 4) * 128, 128, hA, ch, bank)
                        si = rot(k, "sg", 4)
                        S.op("scalar", lambda e, si=si, ps=ps: e.activation(out=sg[si][:], in_=ps[:], func=AF.Sigmoid), r=[ups], w=[usg[si]])
                        ti = rot(k, "tmp", 2)
                        S.op("vector", lambda e, ti=ti, dt=dt, cs=cs, j=j: e.scalar_tensor_tensor(
                            out=k.tmp[ti][:], in0=hms[:, dt, cs], scalar=mhn[:, dt:dt + 1], in1=k.rt[j][:], op0=ALU.mult, op1=ALU.mult),
                             r=[uhms[ch], umhn, k.urt[j]], w=[k.utmp[ti]])
                        S.op("vector", lambda e, ti=ti, si=si, ft=ft, cs=cs: e.tensor_tensor(out=ymA.ap(ft, cs), in0=sg[si][:], in1=k.tmp[ti][:], op=ALU.mult),
                             r=[usg[si], k.utmp[ti]], w=[ymA.u(ft, ch)])
            for grp in range(2):
                wga, uwga = load_wgrp(S, k, wa_d, grp * 512, 512, nbuf=4)
                wgm, uwgm = load_wgrp(S, k, wm_d, grp * 512, 512, nbuf=4)
                wgga, uwgga = load_wgrp(S, k, win_d, 7176 + grp * 512, 512, nbuf=4)
                wggm, uwggm = load_wgrp(S, k, win_d, 8200 + grp * 512, 512, nbuf=4)
                for t in range(4):
                    ft = grp * 4 + t
                    for ch in range(NCH):
                        cs = slice(ch * 512, (ch + 1) * 512)
                        psg, upsg = proj_fm(S, k, wgga, uwgga, t * 128, 128, hA, ch, nb())
                        s1 = rot(k, "sg", 4)
                        S.op("scalar", lambda e, s1=s1, psg=psg: e.activation(out=sg[s1][:], in_=psg[:], func=AF.Sigmoid), r=[upsg], w=[usg[s1]])
                        psa, upsa = proj_fm(S, k, wga, uwga, t * 128, 128, yaA, ch, nb())
                        ta = rot(k, "tmp", 2)
                        S.op("vector", lambda e, ta=ta, s1=s1, psa=psa: e.tensor_tensor(out=k.tmp[ta][:], in0=psa[:], in1=sg[s1][:], op=ALU.mult),
                             r=[upsa, usg[s1]], w=[k.utmp[ta]])
                        psg2, upsg2 = proj_fm(S, k, wggm, uwggm, t * 128, 128, hA, ch, nb())
                        s2 = rot(k, "sg", 4)
                        S.op("scalar", lambda e, s2=s2, psg2=psg2: e.activation(out=sg[s2][:], in_=psg2[:], func=AF.Sigmoid), r=[upsg2], w=[usg[s2]])
                        psm, upsm = proj_fm(S, k, wgm, uwgm, t * 128, 128, ymA, ch, nb())
                        S.op("vector", lambda e, s2=s2, psm=psm: e.tensor_tensor(out=sg[s2][:], in0=psm[:], in1=sg[s2][:], op=ALU.mult),
                             r=[upsm, usg[s2]], w=[usg[s2]])
                        S.op("vector", lambda e, s2=s2, ta=ta, ft=ft, cs=cs: e.tensor_tensor(out=mgA.ap(ft, cs), in0=sg[s2][:], in1=k.tmp[ta][:], op=ALU.add),
                             r=[usg[s2], k.utmp[ta]], w=[mgA.u(ft, ch)])
            for grp in range(2):
                wg, uwg = load_wgrp(S, k, wo_d, grp * 512, 512, nbuf=4)
                for t in range(4):
                    ft = grp * 4 + t
                    for ch in range(NCH):
                        cs = slice(ch * 512, (ch + 1) * 512)
                        ps, ups = proj_fm(S, k, wg, uwg, t * 128, 128, mgA, ch, nb())
                        S.op("vector", lambda e, ps=ps, ft=ft, cs=cs: e.scalar_tensor_tensor(
                            out=xT[:, ft, cs], in0=ps[:], scalar=modT[:, 16 + ft:17 + ft], in1=xT[:, ft, cs], op0=ALU.mult, op1=ALU.add),
                             r=[ups, umod, uxT[ft][ch]], w=[uxT[ft][ch]])
            ada_norm(S, k, xA, NT, Affn, uAffn, modT, umod, 3, h2A, bank=1)
            for fh in range(2):
                for grp in range(4):
                    wg, uwg = load_wgrp(S, k, w1_d, fh * 2048 + grp * 512, 512, nbuf=4)
                    for t in range(4):
                        f = grp * 4 + t
                        for ch in range(NCH):
                            cs = slice(ch * 512, (ch + 1) * 512)
                            ps, ups = proj_fm(S, k, wg, uwg, t * 128, 128, h2A, ch, nb())
                            si = rot(k, "sg", 4)
                            S.op("scalar", lambda e, si=si, ps=ps: e.activation(out=sg[si][:], in_=ps[:], func=AF.Relu), r=[ups], w=[usg[si]])
                            S.op("gpsimd", lambda e, si=si, f=f, cs=cs: e.tensor_tensor(out=uA.ap(f, cs), in0=sg[si][:], in1=sg[si][:], op=ALU.mult),
                                 r=[usg[si]], w=[uA.u(f, ch)])
                for ft in range(8):
                    w2g, uw2g = load_wgrp(S, k, w2_d[fh * 2048:(fh + 1) * 2048, :], ft * 128, 128, nkt=16, name="w2g", nbuf=3, width=128)
                    for ch in range(NCH):
                        cs = slice(ch * 512, (ch + 1) * 512)
                        ps, ups = proj_fm(S, k, w2g, uw2g, 0, 128, uA, ch, nb(), nkt=16)
                        S.op("vector", lambda e, ps=ps, ft=ft, cs=cs: e.scalar_tensor_tensor(
                            out=xT[:, ft, cs], in0=ps[:], scalar=modT[:, 40 + ft:41 + ft], in1=xT[:, ft, cs], op0=ALU.mult, op1=ALU.add),
                             r=[ups, umod, uxT[ft][ch]], w=[uxT[ft][ch]])
            for kt in range(8):
                S.dma("sync", x_o[kt * 128:(kt + 1) * 128, t0:t0 + NT], xT[:, kt, :], r=uxT[kt], w=[uxo[kt]])
        S.finish(uxo)
        S.emit()
    return nc

import ml_dtypes
def fm(v, n):
    return np.ascontiguousarray(v.reshape(n, 128).T)
def A_inputs(x_full, inp, l, NT=2048):
    maps = []
    ncore = x_full.shape[0] // NT
    for i in range(ncore):
        maps.append({
            "xT": np.ascontiguousarray(x_full[i * NT:(i + 1) * NT].T),
            "c_pt": fm(inp["c"][0], 8),
            "w_ada": np.ascontiguousarray(inp["w_ada"][l]),
            "b_ada": fm(inp["b_ada"][l], 48),
            "g_mix": fm(inp["norm_mix_g"][l], 8),
            "w_in": np.ascontiguousarray(inp["w_in"][l]),
            "qg": np.ascontiguousarray(np.tile(inp["qn_g"][l], 2)[:, None]),
            "kg": np.ascontiguousarray(np.tile(inp["kn_g"][l], 2)[:, None]),
        })
    return maps

def t5_bucket_np(dist):
    max_exact = 16
    nf = np.maximum(dist, 1).astype(np.float32)
    large = max_exact + (np.log(nf / max_exact) / np.float32(np.log(128 / max_exact)) * (32 - max_exact)).astype(np.int32)
    large = np.minimum(large, 31)
    return np.where(dist < max_exact, dist, large)

def B1_consts(inp, l, h):
    d = np.arange(256)
    bidx = t5_bucket_np(d)
    bias = inp["rel_table"][bidx, h]
    kk = np.arange(128)[:, None]; qq = np.arange(128)[None, :]
    dist0 = qq - kk
    T0b = np.where(dist0 >= 0, bias[np.clip(dist0, 0, 255)], 0.0).astype(np.float32)
    M0 = np.where(dist0 >= 0, 0.0, -30000.0).astype(np.float32)
    T1 = bias[dist0 + 128].astype(np.float32)
    b31 = np.full((128, 1), inp["rel_table"][31, h], np.float32)
    lam_init = 0.8 - 0.6 * np.exp(-0.3 * l)
    small = np.zeros((1, 512), np.float32)
    small[0, 0:64] = inp["qn_g"][l]; small[0, 64:128] = inp["kn_g"][l]
    small[0, 128:384] = inp["lam_qk"][l].reshape(-1)
    small[0, 384] = lam_init
    return dict(T0b=T0b, M0=M0, T1=T1, b31=b31, small=small, subg=np.ascontiguousarray(inp["subln_g"][l][:, None]))

def B2_inputs(mqT, mkT, mv_tok, gates, inp, l, SEQ):
    NC = SEQ // 64
    maps = []
    utri = np.triu(np.ones((64, 64), np.float32))
    ident = np.eye(128, dtype=np.float32)
    for j in range(8):
        hh, e = j // 2, j % 2
        cwq = inp["conv_w"][l][:, hh * 256:(hh + 1) * 256]
        cwk = inp["conv_w"][l][:, 1024 + hh * 256:1024 + (hh + 1) * 256]
        cw = np.stack([cwq, cwk], 0).reshape(2, 4, 2, 128).transpose(3, 0, 2, 1)
        cbq = inp["conv_b"][l][hh * 256:(hh + 1) * 256]
        cbk = inp["conv_b"][l][1024 + hh * 256:1024 + (hh + 1) * 256]
        cb = np.stack([cbq, cbk], 0).reshape(2, 2, 128).transpose(2, 0, 1)
        gb = np.zeros((64, 2), np.float32); gb[:, 0] = inp["b_igate"][l][hh]; gb[:, 1] = inp["b_fgate"][l][hh]
        maps.append(dict(
            mqT=np.ascontiguousarray(mqT[hh * 256:(hh + 1) * 256]), mkT=np.ascontiguousarray(mkT[hh * 256:(hh + 1) * 256]),
            cw=np.ascontiguousarray(cw), cbias=np.ascontiguousarray(cb),
            mv=np.ascontiguousarray(mv_tok[:, hh * 256 + e * 128: hh * 256 + (e + 1) * 128]),
            gi=np.ascontiguousarray(gates[hh].reshape(NC, 64).T), gf=np.ascontiguousarray(gates[4 + hh].reshape(NC, 64).T),
            gb=gb, utri=utri, ident=ident))
    return maps

def C_inputs(x_full, yaT_full, hmT_full, inp, l, NT=2048):
    maps = []
    ncore = x_full.shape[0] // NT
    for i in range(ncore):
        sl = slice(i * NT, (i + 1) * NT)
        maps.append({
            "xT": np.ascontiguousarray(x_full[sl].T),
            "yaT": np.ascontiguousarray(yaT_full[:, sl]),
            "hmT": np.ascontiguousarray(hmT_full[:, sl]),
            "c_pt": fm(inp["c"][0], 8),
            "w_ada": np.ascontiguousarray(inp["w_ada"][l]),
            "b_ada": fm(inp["b_ada"][l], 48),
            "g_mix": fm(inp["norm_mix_g"][l], 8),
            "g_ffn": fm(inp["norm_ffn_g"][l], 8),
            "mhn": fm(inp["mhn_g"][l], 2),
            "w_in": np.ascontiguousarray(inp["w_in"][l]),
            "w_a": np.ascontiguousarray(inp["w_a"][l]), "w_m": np.ascontiguousarray(inp["w_m"][l]),
            "w_out": np.ascontiguousarray(inp["w_out"][l]),
            "w_ff1": np.ascontiguousarray(inp["w_ff1"][l]), "w_ff2": np.ascontiguousarray(inp["w_ff2"][l]),
        })
    return maps


_PROGS = {}


def _prog(name, fn, *a):
    key = (name,) + a
    if key not in _PROGS:
        _PROGS[key] = fn(*a)
    return _PROGS[key]


def _run(nc, maps):
    res = run_bass_kernel_spmd(nc, maps, core_ids=list(range(8)))
    return res.results


def kernel(**inp):
    inp = {k_: np.asarray(v_) for k_, v_ in inp.items()}
    SEQ = inp["x"].shape[1]
    NTC = SEQ // 8
    x_full = np.ascontiguousarray(inp["x"][0]).astype(np.float32)
    pA = _prog("A", build_A, NTC)
    pB1 = _prog("B1", build_B1, SEQ)
    pB2 = _prog("B2", build_B2, SEQ)
    pC = _prog("C", build_C, NTC)
    for l in range(inp["w_in"].shape[0]):
        ra = _run(pA, A_inputs(x_full, inp, l, NTC))
        qT = np.concatenate([r["qT"] for r in ra], axis=2)
        kT = np.concatenate([r["kT"] for r in ra], axis=2)
        v_full = np.concatenate([r["v_tok"] for r in ra], axis=0)
        mqT = np.concatenate([r["mqT"] for r in ra], axis=1)
        mkT = np.concatenate([r["mkT"] for r in ra], axis=1)
        mv_full = np.concatenate([r["mv_tok"] for r in ra], axis=0)
        gates = np.concatenate([r["gates"] for r in ra], axis=1)
        maps = []
        for h in range(8):
            m = dict(qT=np.ascontiguousarray(qT[h]), kT=np.ascontiguousarray(kT[h]),
                     v=np.ascontiguousarray(v_full[:, h * 128:(h + 1) * 128]))
            m.update(B1_consts(inp, l, h))
            maps.append(m)
        rb1 = _run(pB1, maps)
        yaT = np.concatenate([r["yaT"] for r in rb1], axis=0)
        rb2 = _run(pB2, B2_inputs(mqT, mkT, mv_full, gates, inp, l, SEQ))
        hmT = np.ascontiguousarray(np.concatenate([r["h"] for r in rb2], axis=1).T)
        rc = _run(pC, C_inputs(x_full, yaT, hmT, inp, l, NTC))
        x_full = np.ascontiguousarray(np.concatenate([r["xT_out"] for r in rc], axis=1).T)
    return x_full[None].astype(np.float32)
```
